# Optimizing a Trainium2 kernel written in Bass

```python
import jax, jax.numpy as jnp
from jax import lax
import numpy as np

D_MODEL = 4096
BATCH = 8
SEQ = 2048
DEPTH = 2

D_BRANCH = 1024
N_BRANCH = 3
D_FF = 11008
CHUNK = 64
EPS = 1e-6

M_HEADS = 4
M_DV = D_BRANCH // M_HEADS
M_DK = M_DV // 2
M_CONV = 4
M_GATE_CAP = 15.0

R_HEAD_DIM = 64
R_HEADS = D_BRANCH // R_HEAD_DIM
R_W_RANK = 64
R_A_RANK = 64
R_G_RANK = 128
R_V_RANK = 32
R_GN_EPS = 64e-5

G_HEADS = 4
G_DV = D_BRANCH // G_HEADS
G_DK = G_DV // 2
G_RANK = 16
G_TAU = 16.0

M_SIZES = (M_HEADS * M_DK, M_HEADS * M_DK, D_BRANCH, D_BRANCH, M_HEADS, M_HEADS)
R_SIZES = (D_BRANCH, D_BRANCH, D_BRANCH, R_W_RANK, R_A_RANK, R_G_RANK)
G_SIZES = (G_HEADS * G_DK, G_HEADS * G_DK, D_BRANCH, G_RANK, D_BRANCH)
R_COLS = sum(R_SIZES)
IN_SIZES = M_SIZES + (R_COLS,) + G_SIZES + (N_BRANCH * D_MODEL,)
N_IN = sum(IN_SIZES)
IN_SPLITS = tuple(int(s) for s in np.cumsum(IN_SIZES)[:-1])
R_SPLITS = tuple(int(s) for s in np.cumsum(R_SIZES)[:-1])

kernel_name = "hybrid_mlstm_rwkv7_gla_macaron"


def rmsnorm(x, w):
    xf = x.astype(jnp.float32)
    y = xf * lax.rsqrt(jnp.mean(xf * xf, axis=-1, keepdims=True) + EPS)
    return (y * w.astype(jnp.float32)).astype(x.dtype)


def head_rmsnorm(h, w):
    H, d = h.shape[-2:]
    y = h * lax.rsqrt(jnp.mean(h * h, axis=-1, keepdims=True) + EPS)
    return y * w.reshape(H, d)


def head_layernorm(h, w, b, eps):
    H, d = h.shape[-2:]
    mu = jnp.mean(h, axis=-1, keepdims=True)
    var = jnp.mean(jnp.square(h - mu), axis=-1, keepdims=True)
    return (h - mu) * lax.rsqrt(var + eps) * w.reshape(H, d) + b.reshape(H, d)


def swiglu(x, w_in, w_out):
    gate, up = jnp.split(x @ w_in, 2, axis=-1)
    return (jax.nn.silu(gate) * up) @ w_out


def softcap(x):
    return M_GATE_CAP * jnp.tanh(x / M_GATE_CAP)


def token_shift(z):
    return jnp.pad(z, ((0, 0), (1, 0), (0, 0)))[:, :-1]


def causal_dwconv(z, w):
    K, C = w.shape
    return lax.conv_general_dilated(
        z, w.astype(z.dtype)[:, None, :], window_strides=(1,), padding=[(K - 1, 0)],
        dimension_numbers=('NWC', 'WIO', 'NWC'), feature_group_count=C)


def to_chunks(a):
    B, T, H = a.shape[:3]
    a = a.reshape((B, T // CHUNK, CHUNK, H) + a.shape[3:])
    return jnp.moveaxis(a, (1, 3), (0, 2))


def from_chunks(a):
    a = jnp.moveaxis(a, (0, 2), (1, 3))
    B, NC, L, H = a.shape[:4]
    return a.reshape((B, NC * L, H) + a.shape[4:])


def mlstm_chunkwise(q, k, v, ig, lf):
    B, T, H, DK = q.shape
    DV = v.shape[-1]
    mask = jnp.tril(jnp.ones((CHUNK, CHUNK), dtype=bool))

    def step(carry, inp):
        C, n, m = carry
        qc, kc, vc, ic, lfc = inp
        b = jnp.cumsum(lfc, axis=-1)
        log_d = b[..., :, None] - b[..., None, :] + ic[..., None, :]
        log_d = jnp.where(mask, log_d, -jnp.inf)
        log_inter = b + m[..., None]
        m_t = jnp.maximum(log_inter, jnp.max(log_d, axis=-1))
        d = jnp.exp(log_d - m_t[..., None])
        inter = jnp.exp(log_inter - m_t)
        s = jnp.einsum('bhtk,bhsk->bhts', qc, kc) * d
        num = (jnp.einsum('bhts,bhsv->bhtv', s, vc)
               + inter[..., None] * jnp.einsum('bhtk,bhkv->bhtv', qc, C))
        den = jnp.sum(s, axis=-1) + inter * jnp.einsum('bhtk,bhk->bht', qc, n)
        h = num / jnp.maximum(jnp.abs(den), jnp.exp(-m_t))[..., None]
        log_w = b[..., -1:] - b + ic
        m_new = jnp.maximum(b[..., -1] + m, jnp.max(log_w, axis=-1))
        wk = jnp.exp(log_w - m_new[..., None])
        decay = jnp.exp(b[..., -1] + m - m_new)
        C = decay[..., None, None] * C + jnp.einsum('bhs,bhsk,bhsv->bhkv', wk, kc, vc)
        n = decay[..., None] * n + jnp.einsum('bhs,bhsk->bhk', wk, kc)
        return (C, n, m_new), h

    init = (jnp.zeros((B, H, DK, DV), jnp.float32), jnp.zeros((B, H, DK), jnp.float32),
            jnp.zeros((B, H), jnp.float32))
    _, h = lax.scan(step, init, (to_chunks(q), to_chunks(k), to_chunks(v), to_chunks(ig), to_chunks(lf)))
    return from_chunks(h)


def mlstm_branch(q, k, v, o, ig, fg, conv_w, i_bias, f_bias, norm_w):
    B, T, _ = q.shape
    qk = jax.nn.silu(causal_dwconv(jnp.concatenate([q, k], axis=-1), conv_w))
    qk = qk.astype(jnp.float32).reshape(B, T, 2, M_HEADS, M_DK)
    qh = qk[:, :, 0]
    kh = qk[:, :, 1] * (M_DK ** -0.5)
    vh = v.astype(jnp.float32).reshape(B, T, M_HEADS, M_DV)
    log_i = softcap(ig.astype(jnp.float32) + i_bias)
    log_f = jax.nn.log_sigmoid(softcap(fg.astype(jnp.float32) + f_bias))
    h = mlstm_chunkwise(qh, kh, vh, log_i, log_f)
    h = head_rmsnorm(h, norm_w).reshape(B, T, D_BRANCH)
    return h * jax.nn.sigmoid(o.astype(jnp.float32))


def rwkv7_scan(r, w, k, v, a, b):
    B, T, H, N = r.shape
    xs = tuple(jnp.moveaxis(t, 1, 0) for t in (r, w, k, v, a, b))

    def step(S, inp):
        rt, wt, kt, vt, at, bt = inp
        sa = jnp.einsum('bhvk,bhk->bhv', S, at)
        S = (S * wt[:, :, None, :] + sa[..., None] * bt[:, :, None, :]
             + vt[..., None] * kt[:, :, None, :])
        return S, jnp.einsum('bhvk,bhk->bhv', S, rt)

    _, y = lax.scan(step, jnp.zeros((B, H, N, N), jnp.float32), xs)
    return jnp.moveaxis(y, 0, 1)


def rwkv7_branch(z, mu, w0, w2, a0, a2, g2, k_k, k_a, r_k, ln_w, ln_b, v_first, v_res):
    B, T, _ = z.shape
    z = z.astype(jnp.float32)
    z = z + (token_shift(z) - z) * mu
    r, k, v, zw, za, zg = jnp.split(z, R_SPLITS, axis=-1)
    w_log = -jax.nn.softplus(-(w0 + jnp.tanh(zw) @ w2)) - 0.5
    decay = jnp.exp(-jnp.exp(w_log))
    a = jax.nn.sigmoid(a0 + za @ a2)
    g = jax.nn.sigmoid(zg) @ g2
    if v_res is None:
        v_first = v
    else:
        v0, v1, v2 = v_res
        v = v + (v_first - v) * jax.nn.sigmoid(v0 + (v @ v1) @ v2)

    def heads(t):
        return t.reshape(B, T, R_HEADS, R_HEAD_DIM)

    kk = heads(k * k_k)
    kk = kk / jnp.maximum(jnp.linalg.norm(kk, axis=-1, keepdims=True), 1e-12)
    k = k * (1.0 + (a - 1.0) * k_a)
    rh, kh, vh, ah = heads(r), heads(k), heads(v), heads(a)
    y = rwkv7_scan(rh, heads(decay), kh, vh, -kk, kk * ah)
    y = head_layernorm(y, ln_w, ln_b, R_GN_EPS)
    y = y + jnp.sum(rh * kh * r_k.reshape(R_HEADS, R_HEAD_DIM), axis=-1, keepdims=True) * vh
    return y.reshape(B, T, D_BRANCH) * g, v_first


def gla_chunkwise(q, k, v, log_a):
    B, T, H, DK = q.shape
    DV = v.shape[-1]
    mask = jnp.tril(jnp.ones((CHUNK, CHUNK), dtype=bool))

    def step(S, inp):
        qc, kc, vc, gc = inp
        bc = jnp.cumsum(gc, axis=-2)
        diff = bc[..., :, None, :] - bc[..., None, :, :]
        rel = jnp.exp(jnp.where(mask[..., None], diff, -jnp.inf))
        att = jnp.einsum('bhtk,bhsk,bhtsk->bhts', qc, kc, rel)
        o = (jnp.einsum('bhts,bhsv->bhtv', att, vc)
             + jnp.einsum('bhtk,bhkv->bhtv', qc * jnp.exp(bc), S))
        last = bc[..., -1:, :]
        S = (jnp.exp(last[..., 0, :])[..., None] * S
             + jnp.einsum('bhsk,bhsv->bhkv', kc * jnp.exp(last - bc), vc))
        return S, o

    _, o = lax.scan(step, jnp.zeros((B, H, DK, DV), jnp.float32),
                    (to_chunks(q), to_chunks(k), to_chunks(v), to_chunks(log_a)))
    return from_chunks(o)


def gla_branch(q, k, v, zg, go, gk_up, gk_bias, norm_w):
    B, T, _ = q.shape
    f32 = jnp.float32
    qh = q.astype(f32).reshape(B, T, G_HEADS, G_DK) * (G_DK ** -0.5)
    kh = k.astype(f32).reshape(B, T, G_HEADS, G_DK)
    vh = v.astype(f32).reshape(B, T, G_HEADS, G_DV)
    log_a = jax.nn.log_sigmoid(zg.astype(f32) @ gk_up + gk_bias) / G_TAU
    o = gla_chunkwise(qh, kh, vh, log_a.reshape(B, T, G_HEADS, G_DK))
    o = head_rmsnorm(o, norm_w).reshape(B, T, D_BRANCH)
    return o * jax.nn.silu(go.astype(f32))


def hybrid_mixer(h, w_in, m_conv, m_i_bias, m_f_bias, m_norm,
                 r_mu, r_w0, r_w2, r_a0, r_a2, r_g2, r_k_k, r_k_a, r_r_k, r_ln_w, r_ln_b, r_vres,
                 g_gk_up, g_gk_bias, g_norm, w_branch, w_out, v_first):
    B, T, _ = h.shape
    z = h @ w_in
    mq, mk, mv, mo, mi, mf, rz, gq, gk, gv, gz, go, gates = jnp.split(z, IN_SPLITS, axis=-1)
    y_m = mlstm_branch(mq, mk, mv, mo, mi, mf, m_conv, m_i_bias, m_f_bias, m_norm)
    y_r, v_first = rwkv7_branch(rz, r_mu, r_w0, r_w2, r_a0, r_a2, r_g2, r_k_k, r_k_a, r_r_k,
                                r_ln_w, r_ln_b, v_first, r_vres)
    y_g = gla_branch(gq, gk, gv, gz, go, g_gk_up, g_gk_bias, g_norm)
    gate = jax.nn.sigmoid(gates.astype(jnp.float32)).reshape(B, T, N_BRANCH, D_MODEL)
    merged = (gate[:, :, 0] * (y_m.astype(h.dtype) @ w_branch[0])
              + gate[:, :, 1] * (y_r.astype(h.dtype) @ w_branch[1])
              + gate[:, :, 2] * (y_g.astype(h.dtype) @ w_branch[2]))
    return merged.astype(h.dtype) @ w_out, v_first


def setup_inputs(seed: int = 0) -> dict:
    key = jax.random.key(seed)
    ks = iter(jax.random.split(key, 40))

    def nrm(shape, scale):
        return scale * jax.random.normal(next(ks), shape, jnp.float32)

    def gain(shape):
        return 1.0 + nrm(shape, 0.02)

    L = DEPTH
    return {
        "x": nrm((BATCH, SEQ, D_MODEL), 1.0),
        "ffn1_norm": gain((L, D_MODEL)),
        "ffn1_w_in": nrm((L, D_MODEL, 2 * D_FF), D_MODEL ** -0.5),
        "ffn1_w_out": nrm((L, D_FF, D_MODEL), D_FF ** -0.5),
        "mix_norm": gain((L, D_MODEL)),
        "w_in": nrm((L, D_MODEL, N_IN), D_MODEL ** -0.5),
        "m_conv": nrm((L, M_CONV, 2 * M_HEADS * M_DK), M_CONV ** -0.5),
        "m_i_bias": nrm((L, M_HEADS), 0.1),
        "m_f_bias": 3.0 + nrm((L, M_HEADS), 0.5),
        "m_norm": gain((L, D_BRANCH)),
        "r_mu": jax.random.uniform(next(ks), (L, R_COLS), jnp.float32),
        "r_w0": nrm((L, D_BRANCH), 0.5),
        "r_w2": nrm((L, R_W_RANK, D_BRANCH), 0.5 * R_W_RANK ** -0.5),
        "r_a0": nrm((L, D_BRANCH), 0.1),
        "r_a2": nrm((L, R_A_RANK, D_BRANCH), R_A_RANK ** -0.5),
        "r_g2": nrm((L, R_G_RANK, D_BRANCH), R_G_RANK ** -0.5),
        "r_k_k": 1.0 + nrm((L, D_BRANCH), 0.1),
        "r_k_a": 1.0 + nrm((L, D_BRANCH), 0.1),
        "r_r_k": nrm((L, D_BRANCH), 0.1),
        "r_ln_w": gain((L, D_BRANCH)),
        "r_ln_b": nrm((L, D_BRANCH), 0.02),
        "r_v0": nrm((L - 1, D_BRANCH), 0.1),
        "r_v1": nrm((L - 1, D_BRANCH, R_V_RANK), D_BRANCH ** -0.5),
        "r_v2": nrm((L - 1, R_V_RANK, D_BRANCH), R_V_RANK ** -0.5),
        "g_gk_up": nrm((L, G_RANK, G_HEADS * G_DK), G_RANK ** -0.5),
        "g_gk_bias": nrm((L, G_HEADS * G_DK), 0.1),
        "g_norm": gain((L, D_BRANCH)),
        "w_branch": nrm((L, N_BRANCH, D_BRANCH, D_MODEL), D_BRANCH ** -0.5),
        "w_out": nrm((L, D_MODEL, D_MODEL), D_MODEL ** -0.5),
        "ffn2_norm": gain((L, D_MODEL)),
        "ffn2_w_in": nrm((L, D_MODEL, 2 * D_FF), D_MODEL ** -0.5),
        "ffn2_w_out": nrm((L, D_FF, D_MODEL), D_FF ** -0.5),
        "final_norm": gain((D_MODEL,)),
    }


def reference(x, ffn1_norm, ffn1_w_in, ffn1_w_out, mix_norm, w_in, m_conv, m_i_bias, m_f_bias,
              m_norm, r_mu, r_w0, r_w2, r_a0, r_a2, r_g2, r_k_k, r_k_a, r_r_k, r_ln_w, r_ln_b,
              r_v0, r_v1, r_v2, g_gk_up, g_gk_bias, g_norm, w_branch, w_out,
              ffn2_norm, ffn2_w_in, ffn2_w_out, final_norm):
    v_first = None
    for l in range(DEPTH):
        x = x + 0.5 * swiglu(rmsnorm(x, ffn1_norm[l]), ffn1_w_in[l], ffn1_w_out[l])
        h = rmsnorm(x, mix_norm[l])
        r_vres = None if l == 0 else (r_v0[l - 1], r_v1[l - 1], r_v2[l - 1])
        y, v_first = hybrid_mixer(
            h, w_in[l], m_conv[l], m_i_bias[l], m_f_bias[l], m_norm[l],
            r_mu[l], r_w0[l], r_w2[l], r_a0[l], r_a2[l], r_g2[l], r_k_k[l], r_k_a[l], r_r_k[l],
            r_ln_w[l], r_ln_b[l], r_vres,
            g_gk_up[l], g_gk_bias[l], g_norm[l], w_branch[l], w_out[l], v_first)
        x = x + y.astype(x.dtype)
        x = x + 0.5 * swiglu(rmsnorm(x, ffn2_norm[l]), ffn2_w_in[l], ffn2_w_out[l])
    return rmsnorm(x, final_norm)
```

```python
import contextlib
import numpy as np
import concourse.bass as bass
import concourse.mybir as mybir
from concourse.bass_utils import run_bass_kernel_spmd

F32 = mybir.dt.float32
BF16 = mybir.dt.bfloat16
AF = mybir.ActivationFunctionType
ALU = mybir.AluOpType
AX = mybir.AxisListType

NCORES = 4
NSEQ = 8
CW = 2370
CO = dict(ident=0, ones=128, bones=256, incl=384, strict=448, lower=512, incl16=576, strict16=1088, lower16=1600,
          sel=2112, hsel=2368)
CFG = dict(D=4096, FF=11008, T=2048, L=2)
TT = 512
CH = 64
EPS = 1e-6
DB = 1024
SAME_ENGINE_SYNC = True

M_Q, M_K, M_V, M_O, M_I, M_F = 0, 512, 1024, 2048, 3072, 3076
R_OFF = 3080
R_R, R_K, R_V, R_W, R_A, R_G = 0, 1024, 2048, 3072, 3136, 3200
G_OFF = 3080 + 3328
G_Q, G_K, G_V, G_Z, G_O = 0, 512, 1024, 2048, 2064
GATE_OFF = G_OFF + 3088
N_MIX = GATE_OFF


def mixer_blocks():
    b = []
    for h in range(4): b.append(("mq%d" % h, M_Q + 128 * h, 128))
    for h in range(4): b.append(("mk%d" % h, M_K + 128 * h, 128))
    for i in range(8): b.append(("mv%d" % i, M_V + 128 * i, 128))
    for i in range(8): b.append(("mo%d" % i, M_O + 128 * i, 128))
    b.append(("mi", M_I, 4)); b.append(("mf", M_F, 4))
    for i in range(8): b.append(("rr%d" % i, R_OFF + R_R + 128 * i, 128))
    for i in range(8): b.append(("rk%d" % i, R_OFF + R_K + 128 * i, 128))
    for i in range(8): b.append(("rv%d" % i, R_OFF + R_V + 128 * i, 128))
    b.append(("rw", R_OFF + R_W, 64)); b.append(("ra", R_OFF + R_A, 64)); b.append(("rg", R_OFF + R_G, 128))
    for h in range(4): b.append(("gq%d" % h, G_OFF + G_Q + 128 * h, 128))
    for h in range(4): b.append(("gk%d" % h, G_OFF + G_K + 128 * h, 128))
    for i in range(8): b.append(("gv%d" % i, G_OFF + G_V + 128 * i, 128))
    b.append(("gz", G_OFF + G_Z, 16))
    for i in range(8): b.append(("go%d" % i, G_OFF + G_O + 128 * i, 128))
    return b


def _tile_block(blk, kcb):
    k, n = blk.shape
    out = np.zeros((128, kcb, 128), np.float32)
    out[:, : k // 128, :n] = blk.reshape(k // 128, 128, n).transpose(1, 0, 2)
    return out


def _bank(blocks, kcb):
    nb = len(blocks)
    nbp = (nb + 7) // 8 * 8
    arr = np.zeros((nbp, 128, kcb * 128), np.float32)
    for i, b in enumerate(blocks):
        if b is not None:
            arr[i] = _tile_block(b, kcb).reshape(128, kcb * 128)
    return arr.reshape(nbp * 128, kcb * 128), nbp


def ff_groups(FF):
    ffc = FF // 128
    ng = 4 if ffc >= 4 else 1
    gc = -(-ffc // ng)
    groups = [list(range(g * gc, min((g + 1) * gc, ffc))) for g in range(ng)]
    return ffc, ng, gc, groups


class Prog:
    def __init__(self, nc, stack):
        self.nc = nc
        self.stack = stack
        self.eng = {"pe": nc.tensor, "act": nc.scalar, "dve": nc.vector, "pool": nc.gpsimd, "sp": nc.sync}
        self.sems = {}
        self.cnt = {}
        self.known = {e: {} for e in self.eng}
        self.lastw = {}
        self.readers = {}
        for e in ("pe", "act", "dve", "pool"):
            self._sem(e)

    def _sem(self, key):
        if key not in self.sems:
            self.sems[key] = self.stack.enter_context(self.nc.semaphore("s_%s" % str(key).replace(" ", "")))
            self.cnt[key] = 0
        return self.sems[key]

    def _wait(self, e, ev):
        sem, val = ev
        if self.known[e].get(sem, 0) >= val:
            return
        self.eng[e].wait_ge(self.sems[sem], val)
        self.known[e][sem] = val

    def _deps(self, e, reads, writes):
        evs = {}
        def add(ev):
            if ev is not None and evs.get(ev[0], 0) < ev[1]:
                evs[ev[0]] = ev[1]
        for k in reads:
            add(self.lastw.get(k))
        for k in writes:
            add(self.lastw.get(k))
            for s_, v_ in self.readers.get(k, {}).items():
                add((s_, v_))
        for s_, v_ in evs.items():
            self._wait(e, (s_, v_))

    def _commit(self, ev, reads, writes):
        for k in reads:
            r = self.readers.setdefault(k, {})
            if r.get(ev[0], 0) < ev[1]:
                r[ev[0]] = ev[1]
        for k in writes:
            self.lastw[k] = ev
            self.readers[k] = {}

    def op(self, e, fn, reads=(), writes=()):
        self._deps(e, reads, writes)
        ins = fn(self.eng[e])
        self.cnt[e] += 1
        ins.then_inc(self.sems[e], 1)
        ev = (e, self.cnt[e])
        if e == "pe" or not SAME_ENGINE_SYNC:
            self.known[e][e] = self.cnt[e]
        self._commit(ev, reads, writes)
        return ev

    def mm(self, mms, reads=(), writes=()):
        self._deps("pe", reads, writes)
        ins = None
        for (o, l, r, st, sp_) in mms:
            ins = self.nc.tensor.matmul(o, l, r, start=st, stop=sp_)
        self.cnt["pe"] += 1
        ins.then_inc(self.sems["pe"], 1)
        ev = ("pe", self.cnt["pe"])
        self.known["pe"]["pe"] = self.cnt["pe"]
        self._commit(ev, reads, writes)
        return ev

    def pe(self, fns, reads=(), writes=()):
        self._deps("pe", reads, writes)
        ins = None
        for fn in fns:
            ins = fn(self.nc.tensor)
        self.cnt["pe"] += 1
        ins.then_inc(self.sems["pe"], 1)
        ev = ("pe", self.cnt["pe"])
        self.known["pe"]["pe"] = self.cnt["pe"]
        self._commit(ev, reads, writes)
        return ev

    def tr(self, trs, ident, reads=(), writes=()):
        self._deps("pe", reads, writes)
        ins = None
        for (o, i_) in trs:
            ins = self.nc.tensor.transpose(o, i_, ident)
        self.cnt["pe"] += 1
        ins.then_inc(self.sems["pe"], 1)
        ev = ("pe", self.cnt["pe"])
        self.known["pe"]["pe"] = self.cnt["pe"]
        self._commit(ev, reads, writes)
        return ev

    def dma(self, q, out, in_, semkey, reads=(), writes=()):
        self._deps(q, reads, writes)
        sem = self._sem(("d", semkey))
        ins = self.eng[q].dma_start(out=out, in_=in_)
        ins.then_inc(sem, 16)
        self.cnt[("d", semkey)] += 16
        ev = (("d", semkey), self.cnt[("d", semkey)])
        self._commit(ev, reads, writes)
        return ev

    def wait_all(self, e):
        for key, c in self.cnt.items():
            if c > 0 and not (isinstance(key, tuple) and str(key[1]).startswith("cast_")) and key != "pool":
                self._wait(e, (key, c))

    def barrier(self, engines=("sp", "pe", "dve", "act")):
        for e in engines:
            self.wait_all(e)
        self.lastw = {k: v for k, v in self.lastw.items() if isinstance(k, tuple) and k[0] == "gat"}
        self.readers = {}


def build(cfg, with_mixer=True):
    D, FF, T, L = cfg["D"], cfg["FF"], cfg["T"], cfg["L"]
    KC = D // 128
    NT = T // TT
    FFC, NG, GC, GROUPS = ff_groups(FF)
    MBLK = mixer_blocks()
    NMB = len(MBLK)
    NMBP = (NMB + 7) // 8 * 8

    nc = bass.Bass("TRN2", target_bir_lowering=False)
    st = contextlib.ExitStack()
    P = Prog(nc, st)

    def dram_in(name, shape, dt=F32):
        return nc.dram_tensor(name, list(shape), dt, kind="ExternalInput").ap()

    def sb(name, shape, dt=F32):
        return st.enter_context(nc.sbuf_tensor("sb_" + name, list(shape), dt))

    S = NSEQ // NCORES
    x_in = dram_in("x", [S * T, D])
    out_d = nc.dram_tensor("out", [S * T, D], F32, kind="ExternalOutput").ap()
    consts_in = dram_in("consts", [128, CW])
    cols_in = dram_in("cols", [L, 128, NCOLS(KC)])
    bcast_in = dram_in("bcast", [L, 64, 4 * 1024])
    smat = {}
    for l in range(L):
        smat[l] = dict(
            w2=dram_in("r_w2_%d" % l, [64, 1024]), a2=dram_in("r_a2_%d" % l, [64, 1024]),
            g2=dram_in("r_g2_%d" % l, [128, 1024]), gk=dram_in("g_gk_%d" % l, [16, 512]),
            v1=dram_in("r_v1_%d" % l, [128, 8 * 32]), v2=dram_in("r_v2_%d" % l, [32, 1024]))

    bank_spec = []
    for l in range(L):
        bank_spec += [("f1a%d" % l, KC, _pad8(2 * NG * GC)), ("f1b%d" % l, GC, _pad8(NG * KC)),
                      ("ma%d" % l, KC, NMBP), ("mb%d" % l, KC, _pad8(3 * KC)),
                      ("mc%d" % l, 8, _pad8(3 * KC)), ("md%d" % l, KC, _pad8(KC)),
                      ("f2a%d" % l, KC, _pad8(2 * NG * GC)), ("f2b%d" % l, GC, _pad8(NG * KC))]
    if not with_mixer:
        bank_spec = [b for b in bank_spec if b[0][0] == "f"]
    banks = {}
    for (bn, kcb, nbp) in bank_spec:
        ext = dram_in("w_" + bn, [nbp * 128, kcb * 128])
        gat = nc.dram_tensor("wg_" + bn, [nbp * 128, kcb * 128], BF16)
        banks[bn] = dict(ext=ext, gat=gat, kcb=kcb, nbp=nbp)

    xT = nc.dram_tensor("xT_scr", [KC * 128, T], F32).ap()
    vfs = nc.dram_tensor("vf_scr", [8 * 128, T], F32).ap()

    def xT_ap(c, t0, n=TT):
        return xT[c * 128:(c + 1) * 128, t0:t0 + n]

    consts = sb("consts", [128, CW])
    ident = consts[:, 0:128]
    ones = consts[:, 128:256]
    epsc = sb("epsc", [128, 2])
    psum = [st.enter_context(nc.psum_tensor("ps%d" % i, [128, 512], F32)) for i in range(8)]

    P.dma("sp", consts[:], consts_in[:, :], "consts", writes=["consts"])
    P.op("dve", lambda e: e.memset(epsc[:, 0:1], EPS), writes=["epsc"])
    P.op("dve", lambda e: e.memset(epsc[:, 1:2], 64e-5), writes=["epsc"])

    for (bn, kcb, nbp) in bank_spec:
        b = banks[bn]
        for r0 in range(0, nbp * 128, 128):
            P.dma("pool", b["gat"].ap()[r0:r0 + 128, :], b["ext"][r0:r0 + 128, :], "cast_" + bn, writes=[("gat", bn)])
    for e in ("sp", "pe", "dve", "act"):
        for key, c in P.cnt.items():
            if isinstance(key, tuple) and str(key[1]).startswith("cast_") and c > 0:
                P._wait(e, (key, c))

    def wblocks(bn, b0, m):
        g = banks[bn]["gat"].ap()
        return g[b0 * 128:(b0 + m) * 128, :].rearrange("(m p) f -> p m f", p=128)

    def prologue(seq):
      with contextlib.ExitStack() as ph:
          xin = [ph.enter_context(nc.sbuf_tensor(uname("xin"), [128, D], F32)) for i in range(2)]
          xtr = [ph.enter_context(nc.sbuf_tensor(uname("xtr"), [128, KC, 128], F32)) for i in range(2)]
          for i in range(T // 128):
              s = i % 2
              P.dma("sp", xin[s][:], x_in[seq * T + i * 128:seq * T + (i + 1) * 128, :], ("xin", s), writes=[("xin", s)])
              for q in range(KC // 4):
                  pb = q % 2
                  P.tr([(psum[pb][:, j * 128:(j + 1) * 128], xin[s][:, (4 * q + j) * 128:(4 * q + j + 1) * 128])
                        for j in range(4)], ident, reads=[("xin", s), "consts"], writes=[("ps", pb)])
                  eng = "dve" if q % 2 == 0 else "act"
                  dst = xtr[s][:, 4 * q:4 * q + 4, :]
                  if eng == "dve":
                      P.op("dve", lambda e, d=dst, pb=pb: e.tensor_copy(out=d, in_=psum[pb][:].rearrange("p (a b) -> p a b", a=4)),
                           reads=[("ps", pb)], writes=[("xtr", s)])
                  else:
                      P.op("act", lambda e, d=dst, pb=pb: e.activation(out=d, in_=psum[pb][:].rearrange("p (a b) -> p a b", a=4), func=AF.Copy),
                           reads=[("ps", pb)], writes=[("xtr", s)])
              P.dma("sp", xT[:, i * 128:(i + 1) * 128].rearrange("(c p) t -> p c t", p=128), xtr[s][:],
                    ("xtrst", s), reads=[("xtr", s)], writes=[("xT", i // (TT // 128))])
          P.barrier()

    def norm_tile(ph, t0, tt, gcols, hT, out_f32=None, W=TT):
        xc = ph["xc"]; sq = ph["sq"]; rstd = ph["rstd"]
        for c in range(KC):
            s = c % 4
            P.dma("sp", xc[s][:], xT_ap(c, t0, W), ("xc", s), reads=[("xT", tt)], writes=[("xc", s)])
            s2 = c % 2
            P.op("act", lambda e, s=s, s2=s2: e.activation(out=sq[s2][:], in_=xc[s][:], func=AF.Square),
                 reads=[("xc", s)], writes=[("sq", s2)])
            P.mm([(psum[0][:, 0:W], ones, sq[s2][:], c == 0, c == KC - 1)], reads=[("sq", s2), "consts"], writes=[("ps", 0)])
        P.op("act", lambda e: e.activation(out=rstd[:], in_=psum[0][:, 0:W], func=AF.Sqrt, bias=epsc[:, 0:1], scale=1.0 / D),
             reads=[("ps", 0), "epsc"], writes=["rstd"])
        P.op("dve", lambda e: e.reciprocal(out=rstd[:], in_=rstd[:]), reads=["rstd"], writes=["rstd"])
        for c in range(KC):
            s = c % 4
            P.dma("sp", xc[s][:], xT_ap(c, t0, W), ("xc", s), reads=[("xT", tt)], writes=[("xc", s)])
            dst = hT[:, c, :] if out_f32 is None else out_f32[:, c, :]
            P.op("dve", lambda e, s=s, c=c, dst=dst: e.scalar_tensor_tensor(
                out=dst, in0=xc[s][:], scalar=gcols[:, c:c + 1], in1=rstd[:], op0=ALU.mult, op1=ALU.mult),
                reads=[("xc", s), "rstd", "cols"], writes=["hT"])

    def ffn(l, which):
        bna, bnb = "f%da%d" % (which, l), "f%db%d" % (which, l)
        with contextlib.ExitStack() as phs:
            def psb(name, shape, dt=F32):
                return phs.enter_context(nc.sbuf_tensor(uname(name), list(shape), dt))
            ph = dict(xc=[psb("xc%d" % i, [128, TT]) for i in range(4)], sq=[psb("sq%d" % i, [128, TT]) for i in range(2)],
                      rstd=psb("rstd", [128, TT]))
            hT = psb("hT", [128, KC, TT], BF16)
            aT = psb("aT", [128, GC, TT], BF16)
            cols = psb("colsf", [128, KC])
            w1 = [psb("w1_%d" % i, [128, 2, KC * 128], BF16) for i in range(2)]
            w2 = [psb("w2_%d" % i, [128, GC * 128], BF16) for i in range(3)]
            sg = [psb("sg%d" % i, [128, TT]) for i in range(2)]
            xr = [psb("xr%d" % i, [128, TT]) for i in range(2)]
            coff = COLOFF(KC)["ffn1_norm" if which == 1 else "ffn2_norm"]
            P.dma("sp", cols[:], cols_in[l, :, coff:coff + KC], "cols", writes=["cols"])
            nw1 = nw2 = nxr = 0
            for tt in range(NT):
                t0 = tt * TT
                norm_tile(ph, t0, tt, cols, hT)
                for g in range(NG):
                    nch = len(GROUPS[g])
                    for jj in range(nch):
                        jp = g * GC + jj
                        s = nw1 % 2; nw1 += 1
                        par = jj % 2
                        P.dma("sp", w1[s][:], wblocks(bna, 2 * jp, 2), ("w1", s), reads=[("gat", bna)], writes=[("w1", s)])
                        P.mm([(psum[1 + par][:], w1[s][:, 0, kc * 128:(kc + 1) * 128], hT[:, kc, :], kc == 0, kc == KC - 1)
                              for kc in range(KC)], reads=[("w1", s), "hT"], writes=[("ps", 1 + par)])
                        P.mm([(psum[3 + par][:], w1[s][:, 1, kc * 128:(kc + 1) * 128], hT[:, kc, :], kc == 0, kc == KC - 1)
                              for kc in range(KC)], reads=[("w1", s), "hT"], writes=[("ps", 3 + par)])
                        P.op("act", lambda e, par=par: e.activation(out=sg[par][:], in_=psum[1 + par][:], func=AF.Silu),
                             reads=[("ps", 1 + par)], writes=[("sg", par)])
                        P.op("dve", lambda e, par=par, jj=jj: e.tensor_tensor(out=aT[:, jj, :], in0=sg[par][:], in1=psum[3 + par][:], op=ALU.mult),
                             reads=[("sg", par), ("ps", 3 + par)], writes=["aT"])
                    for d in range(KC):
                        s = nw2 % 3; nw2 += 1
                        par = d % 2
                        P.dma("sp", w2[s][:], wblocks(bnb, g * KC + d, 1)[:, 0, :], ("w2", s), reads=[("gat", bnb)], writes=[("w2", s)])
                        P.mm([(psum[5 + par][:], w2[s][:, kc * 128:(kc + 1) * 128], aT[:, kc, :], kc == 0, kc == nch - 1)
                              for kc in range(nch)], reads=[("w2", s), "aT"], writes=[("ps", 5 + par)])
                        sx = nxr % 2; nxr += 1
                        P.dma("sp", xr[sx][:], xT_ap(d, t0), ("xr", sx), reads=[("xT", tt)], writes=[("xr", sx)])
                        P.op("dve", lambda e, sx=sx, par=par: e.scalar_tensor_tensor(
                            out=xr[sx][:], in0=psum[5 + par][:], scalar=0.5, in1=xr[sx][:], op0=ALU.mult, op1=ALU.add),
                            reads=[("ps", 5 + par), ("xr", sx)], writes=[("xr", sx)])
                        P.dma("sp", xT_ap(d, t0), xr[sx][:], ("xrst", sx), reads=[("xr", sx)], writes=[("xT", tt)])
            P.barrier()

    def epilogue(seq):
        with contextlib.ExitStack() as phs:
            def psb(name, shape, dt=F32):
                return phs.enter_context(nc.sbuf_tensor(uname(name), list(shape), dt))
            ph = dict(xc=[psb("xc%d" % i, [128, TT]) for i in range(4)], sq=[psb("sq%d" % i, [128, TT]) for i in range(2)],
                      rstd=psb("rstd", [128, TT]))
            on = psb("on", [128, KC, TT])
            cols = psb("colsf", [128, KC])
            otm = [psb("otm%d" % i, [128, D]) for i in range(2)]
            coff = COLOFF(KC)["final_norm"]
            P.dma("sp", cols[:], cols_in[0, :, coff:coff + KC], "cols", writes=["cols"])
            no = 0
            for tt in range(NT):
                t0 = tt * TT
                norm_tile(ph, t0, tt, cols, None, out_f32=on)
                for s_ in range(TT // 128):
                    so = no % 2; no += 1
                    for q in range(KC // 4):
                        pb = 1 + q % 2
                        P.tr([(psum[pb][:, j * 128:(j + 1) * 128], on[:, 4 * q + j, s_ * 128:(s_ + 1) * 128]) for j in range(4)],
                             ident, reads=["hT", "consts"], writes=[("ps", pb)])
                        if q % 2 == 0:
                            P.op("dve", lambda e, so=so, q=q, pb=pb: e.tensor_copy(out=otm[so][:, q * 512:(q + 1) * 512], in_=psum[pb][:]),
                                 reads=[("ps", pb)], writes=[("otm", so)])
                        else:
                            P.op("act", lambda e, so=so, q=q, pb=pb: e.activation(out=otm[so][:, q * 512:(q + 1) * 512], in_=psum[pb][:], func=AF.Copy),
                                 reads=[("ps", pb)], writes=[("otm", so)])
                    r0 = seq * T + t0 + s_ * 128
                    P.dma("sp", out_d[r0:r0 + 128, :], otm[so][:], ("ost", so), reads=[("otm", so)], writes=["out"])
            P.barrier()

    for seq in range(S):
        prologue(seq)
        for l in range(L):
            ffn(l, 1)
            if with_mixer:
                MIXER(nc, P, cfg, l, dict(psum=psum, consts=consts, epsc=epsc, cols_in=cols_in, bcast_in=bcast_in,
                                          smat=smat[l], banks=banks, wblocks=wblocks, xT_ap=xT_ap, vfs=vfs,
                                          norm_tile=norm_tile, MBLK=MBLK, st=st))
            ffn(l, 2)
        epilogue(seq)
    st.close()
    return nc, bank_spec


_UID = [0]


def uname(name):
    _UID[0] += 1
    return "sb_%s_%d" % (name, _UID[0])


def _pad8(n):
    return (n + 7) // 8 * 8


def COLOFF(KC):
    names = [("ffn1_norm", KC), ("mix_norm", KC), ("ffn2_norm", KC), ("final_norm", KC),
             ("m_conv", 32), ("m_ib", 1), ("m_fb", 1),
             ("r_mu", 27), ("r_mu1", 27), ("r_w0", 8), ("r_a0", 8), ("r_kk", 8), ("r_ka", 8), ("r_rk", 8), ("r_v0", 8),
             ("g_bias", 4)]
    off = {}
    o = 0
    for n, w in names:
        off[n] = o
        o += w
    off["_total"] = o
    return off


def NCOLS(KC):
    return COLOFF(KC)["_total"]


from_mixer_placeholder = None


def _chunkcol(v, n):
    return np.ascontiguousarray(v.reshape(n, 128).T)


def make_consts():
    c = np.zeros((128, CW), np.float32)
    c[:, 0:128] = np.eye(128)
    c[:, 128:256] = 1.0
    bo = np.zeros((128, 128), np.float32); bo[:64, :64] = 1; bo[64:, 64:] = 1
    c[:, 256:384] = bo
    o = 384
    s_idx = np.arange(64)[:, None]; t_idx = np.arange(64)[None, :]
    incl = (s_idx <= t_idx).astype(np.float32)
    strict = (s_idx < t_idx).astype(np.float32)
    lower = (s_idx > t_idx).astype(np.float32)
    c[:64, o:o + 64] = incl; c[:64, o + 64:o + 128] = strict; c[:64, o + 128:o + 192] = lower
    o += 192
    c[:64, CO["incl16"]:CO["incl16"] + 512] = np.tile(incl, (1, 8))
    c[:64, CO["strict16"]:CO["strict16"] + 512] = np.tile(strict, (1, 8))
    c[:64, CO["lower16"]:CO["lower16"] + 512] = np.tile(lower, (1, 8))
    for h in range(4):
        c[h, CO["sel"] + h * 64: CO["sel"] + (h + 1) * 64] = 1.0
    c[:64, CO["hsel"]] = 1.0
    c[64:, CO["hsel"] + 1] = 1.0
    return c


def prepare(cfg, inp, with_mixer=True):
    D, FF, T, L = cfg["D"], cfg["FF"], cfg["T"], cfg["L"]
    KC = D // 128
    FFC, NG, GC, GROUPS = ff_groups(FF)
    f = lambda a: np.asarray(a, np.float32)
    per_core = [dict() for _ in range(NCORES)]
    shared = {}
    shared["consts"] = make_consts()
    off = COLOFF(KC)
    cols = np.zeros((L, 128, off["_total"]), np.float32)
    bc = np.zeros((L, 64, 4 * 1024), np.float32)
    MB = mixer_blocks()
    for l in range(L):
        cols[l, :, off["ffn1_norm"]:off["ffn1_norm"] + KC] = _chunkcol(f(inp["ffn1_norm"][l]), KC)
        cols[l, :, off["mix_norm"]:off["mix_norm"] + KC] = _chunkcol(f(inp["mix_norm"][l]), KC)
        cols[l, :, off["ffn2_norm"]:off["ffn2_norm"] + KC] = _chunkcol(f(inp["ffn2_norm"][l]), KC)
        cols[l, :, off["final_norm"]:off["final_norm"] + KC] = _chunkcol(f(inp["final_norm"]), KC)
        if with_mixer:
            mc = f(inp["m_conv"][l])
            for j in range(4):
                cols[l, :, off["m_conv"] + 8 * j: off["m_conv"] + 8 * j + 8] = _chunkcol(mc[j], 8)
            cols[l, 0:4, off["m_ib"]] = f(inp["m_i_bias"][l])
            cols[l, 0:4, off["m_fb"]] = f(inp["m_f_bias"][l])
            mu = f(inp["r_mu"][l])
            mup = np.zeros(27 * 128, np.float32)
            mup[0:3072] = mu[0:3072]; mup[3072:3136] = mu[3072:3136]; mup[3200:3264] = mu[3136:3200]; mup[3328:3456] = mu[3200:3328]
            cols[l, :, off["r_mu"]:off["r_mu"] + 27] = _chunkcol(mup, 27)
            cols[l, :, off["r_mu1"]:off["r_mu1"] + 27] = _chunkcol(1.0 - mup, 27) if False else _chunkcol(mup, 27)
            for nm, key in (("r_w0", "r_w0"), ("r_a0", "r_a0"), ("r_kk", "r_k_k"), ("r_ka", "r_k_a"), ("r_rk", "r_r_k")):
                cols[l, :, off[nm]:off[nm] + 8] = _chunkcol(f(inp[key][l]), 8)
            if l >= 1:
                cols[l, :, off["r_v0"]:off["r_v0"] + 8] = _chunkcol(f(inp["r_v0"][l - 1]), 8)
            cols[l, :, off["g_bias"]:off["g_bias"] + 4] = _chunkcol(f(inp["g_gk_bias"][l]), 4)
            bc[l, :, 0:1024] = f(inp["m_norm"][l])[None, :]
            bc[l, :, 1024:2048] = f(inp["g_norm"][l])[None, :]
            bc[l, :, 2048:3072] = f(inp["r_ln_w"][l])[None, :]
            bc[l, :, 3072:4096] = f(inp["r_ln_b"][l])[None, :]
            shared["r_w2_%d" % l] = f(inp["r_w2"][l]); shared["r_a2_%d" % l] = f(inp["r_a2"][l])
            shared["r_g2_%d" % l] = f(inp["r_g2"][l]); shared["g_gk_%d" % l] = f(inp["g_gk_up"][l])
            if l >= 1:
                v1 = f(inp["r_v1"][l - 1])
                shared["r_v1_%d" % l] = np.ascontiguousarray(v1.reshape(8, 128, 32).transpose(1, 0, 2)).reshape(128, 256)
                shared["r_v2_%d" % l] = f(inp["r_v2"][l - 1])
            else:
                shared["r_v1_%d" % l] = np.zeros((128, 256), np.float32)
                shared["r_v2_%d" % l] = np.zeros((32, 1024), np.float32)
        else:
            for nm, shp in (("r_w2", (64, 1024)), ("r_a2", (64, 1024)), ("r_g2", (128, 1024)), ("g_gk", (16, 512)),
                            ("r_v1", (128, 256)), ("r_v2", (32, 1024))):
                shared["%s_%d" % (nm, l)] = np.zeros(shp, np.float32)
        for which, wi, wo in ((1, "ffn1_w_in", "ffn1_w_out"), (2, "ffn2_w_in", "ffn2_w_out")):
            Wi = f(inp[wi][l]); Wo = f(inp[wo][l])
            blocks = []
            for g in range(NG):
                for jj in range(GC):
                    if jj < len(GROUPS[g]):
                        j = GROUPS[g][jj]
                        blocks.append(Wi[:, j * 128:(j + 1) * 128]); blocks.append(Wi[:, FF + j * 128:FF + (j + 1) * 128])
                    else:
                        blocks.append(None); blocks.append(None)
            arr, _ = _bank(blocks, KC)
            for c in range(1): shared["w_f%da%d" % (which, l)] = arr
            blocks = []
            for g in range(NG):
                r0 = GROUPS[g][0] * 128; r1 = (GROUPS[g][-1] + 1) * 128
                for d in range(KC):
                    blocks.append(Wo[r0:r1, d * 128:(d + 1) * 128])
            arr, _ = _bank(blocks, GC)
            for c in range(1): shared["w_f%db%d" % (which, l)] = arr
        if with_mixer:
            Win = f(inp["w_in"][l])
            arr, _ = _bank([Win[:, o:o + n] for (_, o, n) in MB], KC)
            for c in range(1): shared["w_ma%d" % l] = arr
            blocks = []
            for d in range(KC):
                for i in range(3):
                    o = GATE_OFF + i * D + d * 128
                    blocks.append(Win[:, o:o + 128])
            arr, _ = _bank(blocks, KC)
            for c in range(1): shared["w_mb%d" % l] = arr
            Wb = f(inp["w_branch"][l])
            blocks = []
            for d in range(KC):
                for i in range(3):
                    blocks.append(Wb[i][:, d * 128:(d + 1) * 128])
            arr, _ = _bank(blocks, 8)
            for c in range(1): shared["w_mc%d" % l] = arr
            Wout = f(inp["w_out"][l])
            arr, _ = _bank([Wout[:, d * 128:(d + 1) * 128] for d in range(KC)], KC)
            for c in range(1): shared["w_md%d" % l] = arr
    shared["cols"] = cols
    shared["bcast"] = bc
    return per_core, shared


_CACHE = {}


def kernel(**inputs):
    cfg = CFG
    with_mixer = cfg.get("mixer", True)
    x = np.asarray(inputs["x"], np.float32)
    key = (cfg["D"], cfg["FF"], cfg["T"], cfg["L"], with_mixer)
    if key not in _CACHE:
        _CACHE[key] = build(cfg, with_mixer)
    nc, bank_spec = _CACHE[key]
    per_core, shared = prepare(cfg, inputs, with_mixer)
    S = NSEQ // NCORES
    in_maps = []
    for c in range(NCORES):
        m = dict(shared)
        m["x"] = np.ascontiguousarray(x[c * S:(c + 1) * S].reshape(S * cfg["T"], cfg["D"]))
        in_maps.append(m)
    res = run_bass_kernel_spmd(nc, in_maps, core_ids=list(range(NCORES)))
    outs = [np.asarray(res.results[c]["out"]).reshape(S, cfg["T"], cfg["D"]) for c in range(NCORES)]
    return np.concatenate(outs, axis=0).astype(np.float32)


TM = 256
C0 = float(np.exp(-0.5))


def MIXER(nc, P, cfg, l, env):
    D, T, L = cfg["D"], cfg["T"], cfg["L"]
    KC = D // 128
    NTM = T // TM
    NCH = TM // CH
    psum, consts, cols_in, bcast_in = env["psum"], env["consts"], env["cols_in"], env["bcast_in"]
    smat, wblocks, xT_ap, vfs, norm_tile, MBLK = env["smat"], env["wblocks"], env["xT_ap"], env["vfs"], env["norm_tile"], env["MBLK"]
    OFF = COLOFF(KC)
    ident = consts[:, 0:128]
    onesf = consts[:, 128:256]
    bones = consts[:, 256:384]
    incl = consts[0:64, CO["incl"]:CO["incl"] + 64]
    incl8 = consts[0:64, CO["incl16"]:CO["incl16"] + 512]
    strict8 = consts[0:64, CO["strict16"]:CO["strict16"] + 512]
    lower8 = consts[0:64, CO["lower16"]:CO["lower16"] + 512]
    sel = consts[0:4, CO["sel"]:CO["sel"] + 256]
    hsel = consts[:, CO["hsel"]:CO["hsel"] + 2]
    bn_a, bn_b, bn_c, bn_d = "ma%d" % l, "mb%d" % l, "mc%d" % l, "md%d" % l
    DKS = 128 ** -0.5
    import os
    SKIP = set(os.environ.get("MIXSKIP", "").split(","))
    NCH_M = 0 if "m" in SKIP else NCH
    NCH_R = 0 if "r" in SKIP else NCH
    NCH_G = 0 if "g" in SKIP else NCH
    CUT = int(os.environ.get("MIXCUT", "99"))

    def TT_(out, a, b, op, r, w, eng="dve"):
        return P.op(eng, lambda e: e.tensor_tensor(out=out, in0=a, in1=b, op=op), reads=r, writes=w)

    def TS_(out, a, s1, s2, op0, op1, r, w, eng="dve"):
        if s2 is None:
            return P.op(eng, lambda e: e.tensor_scalar(out=out, in0=a, scalar1=s1, scalar2=None, op0=op0), reads=r, writes=w)
        return P.op(eng, lambda e: e.tensor_scalar(out=out, in0=a, scalar1=s1, scalar2=s2, op0=op0, op1=op1), reads=r, writes=w)

    def STT_(out, a, s, b, op0, op1, r, w, accum=None):
        if accum is None:
            return P.op("dve", lambda e: e.scalar_tensor_tensor(out=out, in0=a, scalar=s, in1=b, op0=op0, op1=op1), reads=r, writes=w)
        return P.op("dve", lambda e: e.scalar_tensor_tensor(out=out, in0=a, scalar=s, in1=b, op0=op0, op1=op1, accum_out=accum), reads=r, writes=w)

    def ACT_(out, in_, func, r, w, bias=None, scale=1.0):
        if bias is None:
            return P.op("act", lambda e: e.activation(out=out, in_=in_, func=func, scale=scale), reads=r, writes=w)
        return P.op("act", lambda e: e.activation(out=out, in_=in_, func=func, bias=bias, scale=scale), reads=r, writes=w)

    def CP_(out, in_, r, w, eng="dve"):
        if eng == "act":
            return P.op("act", lambda e: e.activation(out=out, in_=in_, func=AF.Copy), reads=r, writes=w)
        return P.op("dve", lambda e: e.tensor_copy(out=out, in_=in_), reads=r, writes=w)

    def SCAN_(out, d0, d1, init, op0, op1, r, w):
        return P.op("dve", lambda e: e.tensor_tensor_scan(out=out, data0=d0, data1=d1, initial=init, op0=op0, op1=op1), reads=r, writes=w)

    def RECIP_(out, in_, r, w):
        return P.op("dve", lambda e: e.reciprocal(out=out, in_=in_), reads=r, writes=w)

    def MM(out, lhsT, rhs, start=True, stop=True):
        return lambda pe: pe.matmul(out, lhsT, rhs, start=start, stop=stop)

    def TR(out, in_, idn):
        return lambda pe: pe.transpose(out, in_, idn)

    def bc3(ap2, n):
        return ap2.unsqueeze(2).to_broadcast([ap2.shape[0], ap2.shape[1], n])

    with contextlib.ExitStack() as LS:
        def lsb(name, shape, dt=F32):
            return LS.enter_context(nc.sbuf_tensor(uname(name), list(shape), dt))
        hT = lsb("hTm", [128, KC, TM], BF16)
        yT = lsb("yT", [128, 24, TM], BF16)
        cols = lsb("colsm", [128, OFF["_total"]])
        wz = [lsb("wz%d" % i, [128, KC * 128], BF16) for i in range(2)]
        mC = lsb("mC", [128, 4, 257]); mCb = lsb("mCb", [128, 4, 257], BF16)
        gS = lsb("gS", [128, 4, 256]); gSb = lsb("gSb", [128, 4, 256], BF16)
        rH = lsb("rH", [128, 8, 64])
        mcar = lsb("mcar", [128, 8, 3]); rcar = lsb("rcar", [128, 27, 1])
        mst = lsb("mst", [4, 8])
        P.dma("sp", cols[:], cols_in[l, :, :], "colsm", writes=["cols"])
        for t_, k_ in ((mC, "mC"), (mCb, "mCb"), (gS, "gS"), (gSb, "gSb"), (rH, "rH"), (mcar, "mcar"), (rcar, "rcar"), (mst, "mst")):
            P.op("dve", lambda e, t_=t_: e.memset(t_[:], 0.0), writes=[k_])
        TS_(mst[:, 2:3], cols[0:4, OFF["m_ib"]:OFF["m_ib"] + 1], 2.0 / 15, None, ALU.mult, None, ["cols", "mst"], ["mst"])
        TS_(mst[:, 3:4], cols[0:4, OFF["m_fb"]:OFF["m_fb"] + 1], 2.0 / 15, None, ALU.mult, None, ["cols", "mst"], ["mst"])
        nwz = [0]
        P.op("dve", lambda e: e.memset(yT[:], 0.0), writes=["yT"])

        def zblocks(zb, b0, b1, col0):
            for bi in range(b0, b1):
                _, _, n = MBLK[bi]
                s = nwz[0] % 2; nwz[0] += 1
                par = 1 + s
                P.dma("sp", wz[s][:], wblocks(bn_a, bi, 1)[:, 0, :], ("wz", s), reads=[("gat", bn_a)], writes=[("wz", s)])
                P.mm([(psum[par][0:n, 0:TM], wz[s][:, kc * 128:kc * 128 + n], hT[:, kc, :], kc == 0, kc == KC - 1) for kc in range(KC)],
                     reads=[("wz", s), "hT"], writes=[("ps", par)])
                CP_(zb[0:n, bi - b0, col0:col0 + TM], psum[par][0:n, 0:TM], [("ps", par)], ["zb"], eng="dve" if s == 0 else "act")

        def finish_branch(sc, ytm, br, c, key="ytm"):
            P.pe([TR(psum[3][:, j * 64:(j + 1) * 64], ytm[0:64, j * 128:(j + 1) * 128], ident[0:64, 0:64]) for j in range(8)],
                 reads=[key, "consts"], writes=[("ps", 3)])
            CP_(yT[:, 8 * br:8 * br + 8, c * 64:(c + 1) * 64], psum[3][:, 0:512].rearrange("p (j t) -> p j t", j=8),
                [("ps", 3)], ["yT"], eng="act")

        def to_tm(zb, blk0, zs, dst_fn, keyw):
            for hf in range(2):
                pb = 4 + hf
                P.pe([TR(psum[pb][0:64, j * 128:(j + 1) * 128], zb[:, blk0 + 4 * hf + j, zs], ident) for j in range(4)],
                     reads=["zb", "consts"], writes=[("ps", pb)])
                dst_fn(hf, psum[pb][0:64, 0:512], [("ps", pb)], [keyw])

        def head_norm_gate(sc, hbuf, nh, dh, bcn, gate_tm, ytm, eps):
            ss, junk = sc["ss"], sc["junk"]
            for h in range(nh):
                STT_(junk[:, 0:dh], hbuf[:, h, :], 1.0, hbuf[:, h, :], ALU.mult, ALU.mult, ["hbuf"], ["junk", "ss"], accum=ss[:, h:h + 1])
            TS_(ss[:, 0:nh], ss[:, 0:nh], 1.0 / dh, eps, ALU.mult, ALU.add, ["ss"], ["ss"])
            ACT_(ss[:, 0:nh], ss[:, 0:nh], AF.Sqrt, ["ss"], ["ss"])
            RECIP_(ss[:, 0:nh], ss[:, 0:nh], ["ss"], ["ss"])
            for h in range(nh):
                STT_(ytm[:, h * dh:(h + 1) * dh], hbuf[:, h, :], ss[:, h:h + 1], bcn[0:64, h * dh:(h + 1) * dh], ALU.mult, ALU.mult,
                     ["hbuf", "ss", "bcn"], ["ytm"])
            TT_(ytm[:], ytm[:], gate_tm[:], ALU.mult, ["ytm", "gtm"], ["ytm"])

        for tm in range(NTM):
            t0 = tm * TM
            xkey = t0 // TT
            with contextlib.ExitStack() as S0:
                ph = dict(xc=[S0.enter_context(nc.sbuf_tensor(uname("xc"), [128, TM], F32)) for i in range(4)],
                          sq=[S0.enter_context(nc.sbuf_tensor(uname("sq"), [128, TM], F32)) for i in range(2)],
                          rstd=S0.enter_context(nc.sbuf_tensor(uname("rstd"), [128, TM], F32)))
                norm_tile(ph, t0, xkey, cols[:, OFF["mix_norm"]:OFF["mix_norm"] + KC], hT, W=TM)
                P.barrier()

            with contextlib.ExitStack() as S1:
                def a_(name, shape, dt=F32):
                    return S1.enter_context(nc.sbuf_tensor(uname(name), list(shape), dt))
                zb = a_("zbm", [128, 26, TM + 4])
                P.op("dve", lambda e: e.memset(zb[:], 0.0), writes=["zb"])
                acc = a_("acc", [128, 8, TM]); qkf = a_("qkf", [128, 8, TM]); qkb = a_("qkb", [128, 8, TM], BF16)
                bcn = a_("bcnm", [64, 1024])
                g_t1 = a_("g_t1", [4, TM]); g_lf = a_("g_lf", [4, TM]); g_B = a_("g_B", [4, TM]); g_G = a_("g_G", [4, TM]); g_M = a_("g_M", [4, TM])
                g_S = a_("g_S", [4, NCH]); g_nE = a_("g_nE", [4, NCH])
                c4 = [a_("c4_%d" % i, [4, 64]) for i in range(5)]
                dec = a_("dec", [4, 1]); ddg = a_("ddg", [4, 4])
                gt = a_("gt", [64, 16]); decb = a_("decb", [128, 4])
                vtm = a_("vtm", [64, 4, 257], BF16); sotm = a_("sotm", [64, 1024])
                khat = a_("khat", [64, 128], BF16); arg = a_("arg", [64, 64]); Dm = a_("Dm", [64, 64]); pt = a_("pt", [64, 64])
                ptb = a_("ptb", [64, 64], BF16); nsb = a_("nsb", [64, 257]); comb = a_("comb", [64, 257])
                ab = a_("ab", [64, 1]); hbuf = a_("hbuf", [64, 4, 256]); ytm = a_("ytm", [64, 1024])
                sc = dict(ss=a_("ss", [64, 16]), junk=a_("junk", [64, 256]))
                P.dma("sp", bcn[:], bcast_in[l, :, 0:1024], "bcn", writes=["bcn"])
                P.op("dve", lambda e: e.memset(vtm[:, :, 256:257], 1.0), writes=["vtm"])
                CP_(zb[:, 0:8, 0:3], mcar[:], ["mcar"], ["zb"])
                zblocks(zb, 0, 26, 3)
                CP_(mcar[:], zb[:, 0:8, TM:TM + 3], ["zb"], ["mcar"])
                cw = OFF["m_conv"]
                for blk in range(8):
                    TS_(acc[:, blk, :], zb[:, blk, 0:TM], cols[:, cw + blk:cw + blk + 1], None, ALU.mult, None, ["zb", "cols"], ["acc"])
                    for j in range(1, 4):
                        STT_(acc[:, blk, :], zb[:, blk, j:j + TM], cols[:, cw + 8 * j + blk:cw + 8 * j + blk + 1], acc[:, blk, :],
                             ALU.mult, ALU.add, ["zb", "cols", "acc"], ["acc"])
                ACT_(qkf[:], acc[:], AF.Silu, ["acc"], ["qkf"])
                CP_(qkb[:], qkf[:], ["qkf"], ["qkb"])
                ACT_(g_t1[:], zb[0:4, 24, 3:3 + TM], AF.Sigmoid, ["zb", "mst"], ["g_t1"], bias=mst[:, 2:3], scale=2.0 / 15)
                TS_(g_t1[:], g_t1[:], 2.0, -1.0, ALU.mult, ALU.add, ["g_t1"], ["g_t1"])
                ACT_(g_lf[:], zb[0:4, 25, 3:3 + TM], AF.Sigmoid, ["zb", "mst"], ["g_lf"], bias=mst[:, 3:4], scale=2.0 / 15)
                TS_(g_lf[:], g_lf[:], 2.0, -1.0, ALU.mult, ALU.add, ["g_lf"], ["g_lf"])
                ACT_(g_lf[:], g_lf[:], AF.Sigmoid, ["g_lf"], ["g_lf"], scale=15.0)
                ACT_(g_lf[:], g_lf[:], AF.Ln, ["g_lf"], ["g_lf"])
                ones4 = a_("ones4", [4, TM])
                P.op("dve", lambda e: e.memset(ones4[:], 1.0), writes=["ones4"])
                SCAN_(g_B[:], ones4[:], g_lf[:], mst[:, 0:1], ALU.mult, ALU.add, ["ones4", "g_lf", "mst"], ["g_B"])
                STT_(g_G[:], g_t1[:], 15.0, g_B[:], ALU.mult, ALU.subtract, ["g_t1", "g_B"], ["g_G"])
                SCAN_(g_M[:], g_G[:], g_G[:], mst[:, 1:2], ALU.max, ALU.max, ["g_G", "mst"], ["g_M"])
                CP_(g_S[:, 0:1], mst[:, 1:2], ["mst"], ["g_S"])
                for c in range(1, NCH):
                    CP_(g_S[:, c:c + 1], g_M[:, 64 * c - 1:64 * c], ["g_M"], ["g_S"])
                for c in range(NCH):
                    TS_(g_nE[:, c:c + 1], g_M[:, 64 * c + 63:64 * c + 64], -1.0, None, ALU.mult, None, ["g_M"], ["g_nE"])
                CP_(mst[:, 0:1], g_B[:, TM - 1:TM], ["g_B", "g_S"], ["mst"])
                CP_(mst[:, 1:2], g_M[:, TM - 1:TM], ["g_M", "g_S"], ["mst"])
                for c in range(NCH_M):
                    cs = slice(64 * c, 64 * c + 64)
                    zs = slice(3 + 64 * c, 3 + 64 * c + 64)
                    ia, enm, wk, nG, tmp4 = c4
                    TS_(ia[:], g_M[:, cs], g_S[:, c:c + 1], -1.0, ALU.subtract, ALU.mult, ["g_M", "g_S"], ["c4a"])
                    ACT_(ia[:], ia[:], AF.Exp, ["c4a"], ["c4a"])
                    TT_(tmp4[:], g_B[:, cs], g_M[:, cs], ALU.add, ["g_B", "g_M"], ["c4e"])
                    ACT_(enm[:], tmp4[:], AF.Exp, ["c4e"], ["c4b"], scale=-1.0)
                    ACT_(wk[:], g_G[:, cs], AF.Exp, ["g_G", "g_nE"], ["c4c"], bias=g_nE[:, c:c + 1])
                    ACT_(dec[:], g_S[:, c:c + 1], AF.Exp, ["g_S", "g_nE"], ["dec"], bias=g_nE[:, c:c + 1])
                    TS_(nG[:], g_M[:, cs], -1.0, None, ALU.mult, None, ["g_M"], ["c4d"])
                    TS_(ddg[:], ident[0:4, 0:4], dec[:, 0:1], None, ALU.mult, None, ["dec", "consts"], ["ddg"])
                    P.pe([TR(psum[3][0:64, 0:4], g_G[:, cs], ident[0:4, 0:4]), TR(psum[3][0:64, 4:8], ia[:], ident[0:4, 0:4]),
                          TR(psum[3][0:64, 8:12], enm[:], ident[0:4, 0:4]), TR(psum[3][0:64, 12:16], wk[:], ident[0:4, 0:4]),
                          MM(psum[3][:, 16:20], onesf[0:4, :], ddg[:])],
                         reads=["g_G", "c4a", "c4b", "c4c", "ddg", "consts"], writes=[("ps", 3)])
                    CP_(gt[:], psum[3][0:64, 0:16], [("ps", 3)], ["gt"])
                    CP_(decb[:], psum[3][:, 16:20], [("ps", 3)], ["decb"])
                    if CUT <= 1: continue
                    to_tm(zb, 8, zs, lambda hf, ps_, r, w: CP_(vtm[:, 2 * hf:2 * hf + 2, 0:256], ps_.rearrange("p (h d) -> p h d", h=2), r, w, eng="act"), "vtm")
                    to_tm(zb, 16, zs, lambda hf, ps_, r, w: ACT_(sotm[:, 512 * hf:512 * hf + 512], ps_, AF.Sigmoid, r, w), "gtm")
                    if CUT <= 2: continue
                    for h in range(4 if CUT > 3 else 0):
                        P.pe([TR(psum[6][0:64, 0:128], qkf[:, 4 + h, cs], ident),
                              MM(psum[6][0:64, 128:192], qkb[:, 4 + h, cs], qkb[:, h, cs]),
                              MM(psum[6][0:64, 192:256], sel[:, h * 64:(h + 1) * 64], nG[:])],
                             reads=["qkf", "qkb", "c4d", "consts"], writes=[("ps", 6)])
                        TS_(khat[:], psum[6][0:64, 0:128], gt[:, 12 + h:13 + h], DKS, ALU.mult, ALU.mult, [("ps", 6), "gt"], ["khat"])
                        TS_(arg[:], psum[6][0:64, 192:256], gt[:, h:h + 1], 0.0, ALU.add, ALU.min, [("ps", 6), "gt"], ["arg"])
                        ACT_(Dm[:], arg[:], AF.Exp, ["arg"], ["Dm"])
                        STT_(pt[:], psum[6][0:64, 128:192], DKS, Dm[:], ALU.mult, ALU.mult, [("ps", 6), "Dm"], ["pt"])
                        TT_(ptb[:], pt[:], incl, ALU.mult, ["pt", "consts"], ["ptb"])
                        P.pe([MM(psum[7][0:64, 0:257], ptb[:], vtm[:, h, :])], reads=["ptb", "vtm"], writes=[("ps", 7)])
                        CP_(nsb[:], psum[7][0:64, 0:257], [("ps", 7)], ["nsb"], eng="act")
                        P.pe([MM(psum[7][0:64, 0:257], qkb[:, h, cs], mCb[:, h, :])], reads=["qkb", "mCb"], writes=[("ps", 7)])
                        STT_(comb[:], psum[7][0:64, 0:257], gt[:, 4 + h:5 + h], nsb[:], ALU.mult, ALU.add, [("ps", 7), "gt", "nsb"], ["comb"])
                        STT_(ab[:], comb[:, 256:257], -1.0, comb[:, 256:257], ALU.mult, ALU.max, ["comb"], ["ab"])
                        TS_(ab[:], ab[:], gt[:, 8 + h:9 + h], None, ALU.max, None, ["ab", "gt"], ["ab"])
                        RECIP_(ab[:], ab[:], ["ab"], ["ab"])
                        TS_(hbuf[:, h, :], comb[:, 0:256], ab[:, 0:1], None, ALU.mult, None, ["comb", "ab"], ["hbuf"])
                        P.pe([MM(psum[6][:, 0:257], khat[:], vtm[:, h, :])], reads=["khat", "vtm"], writes=[("ps", 6)])
                        STT_(mC[:, h, :], mC[:, h, :], decb[:, h:h + 1], psum[6][:, 0:257], ALU.mult, ALU.add, ["mC", "decb", ("ps", 6)], ["mC"])
                        CP_(mCb[:, h, :], mC[:, h, :], ["mC"], ["mCb"], eng="act")
                    if CUT <= 4: continue
                    head_norm_gate(sc, hbuf, 4, 256, bcn, sotm, ytm, EPS)
                    finish_branch(sc, ytm, 0, c)
                P.barrier()

            with contextlib.ExitStack() as S2:
                def a_(name, shape, dt=F32):
                    return S2.enter_context(nc.sbuf_tensor(uname(name), list(shape), dt))
                zb = a_("zbr", [128, 27, TM + 4])
                P.op("dve", lambda e: e.memset(zb[:], 0.0), writes=["zb"])
                w2 = a_("w2", [64, 1024]); a2 = a_("a2", [64, 1024]); g2 = a_("g2", [128, 1024])
                v1 = a_("v1", [128, 256]); v2 = a_("v2", [32, 1024])
                lnw = a_("lnw", [64, 1024]); lnb = a_("lnb", [64, 1024])
                zm = a_("zm", [128, 27, 64])
                F = [a_("rf%d" % i, [128, 8, 64]) for i in range(13)]
                atm = [a_("atm%d" % i, [128, 8, 64]) for i in range(2)]; rtm = [a_("rtm%d" % i, [128, 8, 64]) for i in range(2)]
                tw = a_("tw", [64, 64]); sgz = a_("sgz", [128, 64]); t1sb = a_("t1sb", [32, 64]); vf = a_("vf", [128, 8, 64])
                bon = a_("bon", [64, 16])
                Vtm = a_("Vtm", [64, 1024]); bhtm = a_("bhtm", [64, 1024]); khtm = a_("khtm", [64, 1024]); gtm = a_("gtm", [64, 1024])
                Ysb = a_("Ysb", [64, 1024]); ytm = bhtm; yc = khtm
                Xa = [a_("Xa%d" % i, [64, 512]) for i in range(2)]; XTa = [a_("XTa%d" % i, [64, 512]) for i in range(2)]
                Wa = [a_("Wa%d" % i, [64, 512]) for i in range(2)]
                MakT = a_("MakT", [64, 512]); MrbT = a_("MrbT", [64, 512]); MrkT = a_("MrkT", [64, 512])
                st16 = a_("st16", [64, 16]); st16b = a_("st16b", [64, 16])
                for t_, src, k_ in ((w2, smat["w2"], "w2"), (a2, smat["a2"], "a2"), (g2, smat["g2"], "g2"), (v1, smat["v1"], "v1"), (v2, smat["v2"], "v2")):
                    P.dma("sp", t_[:], src[:, :], k_, writes=[k_])
                P.dma("sp", lnw[:], bcast_in[l, :, 2048:3072], "lnw", writes=["lnw"])
                P.dma("sp", lnb[:], bcast_in[l, :, 3072:4096], "lnb", writes=["lnb"])
                CP_(zb[:, :, 2:3], rcar[:], ["rcar"], ["zb"])
                zblocks(zb, 26, 53, 3)
                CP_(rcar[:], zb[:, :, TM + 2:TM + 3], ["zb"], ["rcar"])
                cb = lambda nm, n=8: bc3(cols[:, OFF[nm]:OFF[nm] + n], 64)
                (sw, asig, cl, epos, eneg, eex, kk, kkn, kmod, at, bt, kt, rt) = F
                bb, bh, kh = eex, sw, cl
                for c in range(NCH_R):
                    zs = slice(3 + 64 * c, 3 + 64 * c + 64)
                    zp = slice(2 + 64 * c, 2 + 64 * c + 64)
                    TT_(zm[:], zb[:, :, zp], zb[:, :, zs], ALU.subtract, ["zb"], ["zm"])
                    TT_(zm[:], zm[:], cb("r_mu", 27), ALU.mult, ["zm", "cols"], ["zm"])
                    TT_(zm[:], zm[:], zb[:, :, zs], ALU.add, ["zm", "zb"], ["zm"])
                    r_, k_, v_ = zm[:, 0:8, :], zm[:, 8:16, :], zm[:, 16:24, :]
                    ACT_(tw[:], zm[0:64, 24, :], AF.Sigmoid, ["zm"], ["tw"], scale=2.0)
                    TS_(tw[:], tw[:], 2.0, -1.0, ALU.mult, ALU.add, ["tw"], ["tw"])
                    P.pe([MM(psum[3][:, b * 64:(b + 1) * 64], w2[:, b * 128:(b + 1) * 128], tw[:]) for b in range(8)], reads=["w2", "tw"], writes=[("ps", 3)])
                    TT_(sw[:], psum[3][:, 0:512].rearrange("p (a b) -> p a b", a=8), cb("r_w0"), ALU.add, [("ps", 3), "cols"], ["sw"])
                    ACT_(sw[:], sw[:], AF.Sigmoid, ["sw"], ["sw"])
                    P.pe([MM(psum[3][:, b * 64:(b + 1) * 64], a2[:, b * 128:(b + 1) * 128], zm[0:64, 25, :]) for b in range(8)], reads=["a2", "zm"], writes=[("ps", 3)])
                    TT_(asig[:], psum[3][:, 0:512].rearrange("p (a b) -> p a b", a=8), cb("r_a0"), ALU.add, [("ps", 3), "cols"], ["asig"])
                    ACT_(asig[:], asig[:], AF.Sigmoid, ["asig"], ["asig"])
                    for b in range(8):
                        SCAN_(cl[:, b, :], onesf[:, 0:64], sw[:, b, :], 0.0, ALU.mult, ALU.add, ["sw", "consts"], ["cl"])
                    ACT_(epos[:], cl[:], AF.Exp, ["cl"], ["epos"], scale=-C0)
                    ACT_(eneg[:], cl[:], AF.Exp, ["cl"], ["eneg"], scale=C0)
                    TT_(eex[:], cl[:], sw[:], ALU.subtract, ["cl", "sw"], ["eex"])
                    ACT_(eex[:], eex[:], AF.Exp, ["eex"], ["eex"], scale=-C0)
                    TT_(kk[:], k_, cb("r_kk"), ALU.mult, ["zm", "cols"], ["kk"])
                    TT_(kkn[:], kk[:], kk[:], ALU.mult, ["kk"], ["kkn"])
                    P.pe([MM(psum[3][:, 0:512], bones, kkn[:].rearrange("p a b -> p (a b)"))], reads=["kkn", "consts"], writes=[("ps", 3)])
                    ACT_(kkn[:], psum[3][:, 0:512].rearrange("p (a b) -> p a b", a=8), AF.Sqrt, [("ps", 3)], ["kkn"])
                    TS_(kkn[:], kkn[:], 1e-12, None, ALU.max, None, ["kkn"], ["kkn"])
                    RECIP_(kkn[:], kkn[:], ["kkn"], ["kkn"])
                    TT_(kkn[:], kk[:], kkn[:], ALU.mult, ["kk", "kkn"], ["kkn"])
                    STT_(kmod[:], asig[:], -1.0, cb("r_ka"), ALU.add, ALU.mult, ["asig", "cols"], ["kmod"])
                    STT_(kmod[:], kmod[:], 1.0, k_, ALU.add, ALU.mult, ["kmod", "zm"], ["kmod"])
                    STT_(at[:], kkn[:], -1.0, eex[:], ALU.mult, ALU.mult, ["kkn", "eex"], ["at"])
                    TT_(bb[:], kkn[:], asig[:], ALU.mult, ["kkn", "asig", "at", "eex"], ["eex"])
                    TT_(bt[:], bb[:], eneg[:], ALU.mult, ["eex", "eneg"], ["bt"])
                    TT_(kt[:], kmod[:], eneg[:], ALU.mult, ["kmod", "eneg"], ["kt"])
                    TT_(rt[:], r_, epos[:], ALU.mult, ["zm", "epos"], ["rt"])
                    for hf in range(2):
                        TS_(atm[hf][:], at[:], hsel[:, hf:hf + 1], None, ALU.mult, None, ["at", "consts"], ["atm"])
                        TS_(rtm[hf][:], rt[:], hsel[:, hf:hf + 1], None, ALU.mult, None, ["rt", "consts"], ["rtm"])
                    glb = epos[:, :, 63:64].to_broadcast([128, 8, 64])
                    TT_(bh[:], bt[:], glb, ALU.mult, ["bt", "epos", "sw"], ["sw"])
                    TT_(kh[:], kt[:], glb, ALU.mult, ["kt", "epos", "cl", "eex"], ["cl"])
                    TT_(kk[:], r_, kmod[:], ALU.mult, ["zm", "kmod", "kk"], ["kk"])
                    TT_(kk[:], kk[:], cb("r_rk"), ALU.mult, ["kk", "cols"], ["kk"])
                    P.pe([MM(psum[4][0:64, 2 * b:2 * b + 2], kk[:, b, :], hsel) for b in range(8)], reads=["kk", "consts"], writes=[("ps", 4)])
                    CP_(bon[:], psum[4][0:64, 0:16], [("ps", 4)], ["bon"])
                    vdram = vfs[:, t0 + 64 * c:t0 + 64 * c + 64].rearrange("(b p) t -> p b t", p=128)
                    if l == 0:
                        CP_(vf[:], v_, ["zm"], ["vf"])
                        P.dma("sp", vdram, vf[:], "vfst", reads=["vf"], writes=[("vfs", tm)])
                        vv = vf
                    else:
                        P.dma("sp", vf[:], vdram, "vfld", reads=[("vfs", tm)], writes=["vf"])
                        P.pe([MM(psum[4][0:32, 64:128], v1[:, b * 32:(b + 1) * 32], zm[:, 16 + b, :], start=(b == 0), stop=(b == 7)) for b in range(8)],
                             reads=["v1", "zm"], writes=[("ps", 4)])
                        CP_(t1sb[:], psum[4][0:32, 64:128], [("ps", 4)], ["t1sb"])
                        P.pe([MM(psum[3][:, b * 64:(b + 1) * 64], v2[:, b * 128:(b + 1) * 128], t1sb[:]) for b in range(8)], reads=["v2", "t1sb"], writes=[("ps", 3)])
                        TT_(kk[:], psum[3][:, 0:512].rearrange("p (a b) -> p a b", a=8), cb("r_v0"), ALU.add, [("ps", 3), "cols", "kk"], ["kk"])
                        ACT_(kk[:], kk[:], AF.Sigmoid, ["kk"], ["kk"])
                        TT_(vf[:], vf[:], v_, ALU.subtract, ["vf", "zm"], ["vf"])
                        TT_(vf[:], vf[:], kk[:], ALU.mult, ["vf", "kk"], ["vf"])
                        TT_(vf[:], vf[:], v_, ALU.add, ["vf", "zm"], ["vf"])
                        vv = vf
                    for (srcb, dst, kd) in ((vv, Vtm, "Vtm"), (bh, bhtm, "bhtm"), (kh, khtm, "khtm")):
                        for hf in range(2):
                            pb = 4 + hf
                            P.pe([TR(psum[pb][0:64, j * 128:(j + 1) * 128], srcb[:, 4 * hf + j, :], ident) for j in range(4)],
                                 reads=["vf", "sw", "cl", "consts"], writes=[("ps", pb)])
                            CP_(dst[:, 512 * hf:512 * hf + 512], psum[pb][0:64, 0:512], [("ps", pb)], [kd], eng="act" if hf else "dve")
                    ACT_(sgz[:], zm[:, 26, :], AF.Sigmoid, ["zm"], ["sgz"])
                    for hf in range(2):
                        pb = 4 + hf
                        P.pe([MM(psum[pb][0:64, 0:512], sgz[:], g2[:, 512 * hf:512 * hf + 512])], reads=["sgz", "g2"], writes=[("ps", pb)])
                        CP_(gtm[:, 512 * hf:512 * hf + 512], psum[pb][0:64, 0:512], [("ps", pb)], ["gtm"], eng="act" if hf else "dve")
                    for hg in range(2):
                        def hd_(i):
                            hd = 8 * hg + i
                            return hd, hd // 2, slice((hd % 2) * 64, (hd % 2) * 64 + 64)
                        def o_(pb, i):
                            return psum[pb][0:64, i * 64:(i + 1) * 64]
                        def fm_(t_, i):
                            hd, b, rows = hd_(i)
                            return t_[:, b, :]
                        def mk_(tm_, i):
                            hd, b, rows = hd_(i)
                            return tm_[hd % 2][:, b, :]
                        P.pe([MM(o_(3, i), fm_(bt, i), mk_(atm, i)) for i in range(8)], reads=["bt", "atm"], writes=[("ps", 3)])
                        TT_(XTa[0][:], psum[3][0:64, 0:512], strict8, ALU.mult, [("ps", 3), "consts"], ["XT0"])
                        P.pe([MM(o_(4, i), mk_(atm, i), fm_(bt, i)) for i in range(8)], reads=["bt", "atm"], writes=[("ps", 4)])
                        TT_(Xa[0][:], psum[4][0:64, 0:512], lower8, ALU.mult, [("ps", 4), "consts"], ["X0"])
                        P.pe([MM(o_(5, i), fm_(kt, i), mk_(atm, i)) for i in range(8)], reads=["kt", "atm"], writes=[("ps", 5)])
                        TT_(MakT[:], psum[5][0:64, 0:512], strict8, ALU.mult, [("ps", 5), "consts"], ["MakT"])
                        P.pe([MM(o_(6, i), fm_(bt, i), mk_(rtm, i)) for i in range(8)], reads=["bt", "rtm"], writes=[("ps", 6)])
                        TT_(MrbT[:], psum[6][0:64, 0:512], incl8, ALU.mult, [("ps", 6), "consts"], ["MrbT"])
                        P.pe([MM(o_(7, i), fm_(kt, i), mk_(rtm, i)) for i in range(8)], reads=["kt", "rtm"], writes=[("ps", 7)])
                        TT_(MrkT[:], psum[7][0:64, 0:512], incl8, ALU.mult, [("ps", 7), "consts"], ["MrkT"])
                        P.pe([MM(o_(3, i), mk_(atm, i), fm_(rH, i)) for i in range(8)], reads=["atm", "rH"], writes=[("ps", 3)])
                        CP_(Wa[0][:], psum[3][0:64, 0:512], [("ps", 3)], ["W0"], eng="act")
                        P.pe([MM(o_(4, i), MakT[:, i * 64:(i + 1) * 64], Vtm[:, hd_(i)[0] * 64:(hd_(i)[0] + 1) * 64]) for i in range(8)],
                             reads=["MakT", "Vtm"], writes=[("ps", 4)])
                        TT_(Wa[0][:], Wa[0][:], psum[4][0:64, 0:512], ALU.add, ["W0", ("ps", 4)], ["W0"])
                        for j in range(6):
                            cur, nxt = j % 2, (j + 1) % 2
                            P.pe([MM(o_(4, i), XTa[cur][:, i * 64:(i + 1) * 64], Wa[cur][:, i * 64:(i + 1) * 64]) for i in range(8)],
                                 reads=["XT%d" % cur, "W%d" % cur], writes=[("ps", 4)])
                            TT_(Wa[nxt][:], Wa[cur][:], psum[4][0:64, 0:512], ALU.add, ["W%d" % cur, ("ps", 4)], ["W%d" % nxt])
                            if j < 5:
                                P.pe([MM(o_(5, i), XTa[cur][:, i * 64:(i + 1) * 64], Xa[cur][:, i * 64:(i + 1) * 64]) for i in range(8)],
                                     reads=["XT%d" % cur, "X%d" % cur], writes=[("ps", 5)])
                                P.pe([MM(o_(6, i), Xa[cur][:, i * 64:(i + 1) * 64], XTa[cur][:, i * 64:(i + 1) * 64]) for i in range(8)],
                                     reads=["XT%d" % cur, "X%d" % cur], writes=[("ps", 6)])
                                CP_(Xa[nxt][:], psum[5][0:64, 0:512], [("ps", 5)], ["X%d" % nxt], eng="act")
                                CP_(XTa[nxt][:], psum[6][0:64, 0:512], [("ps", 6)], ["XT%d" % nxt], eng="dve")
                        U = Wa[0]
                        P.pe([MM(o_(7, i), mk_(rtm, i), fm_(rH, i)) for i in range(8)], reads=["rtm", "rH"], writes=[("ps", 7)])
                        CP_(Ysb[:, 512 * hg:512 * hg + 512], psum[7][0:64, 0:512], [("ps", 7)], ["Ysb"], eng="act")
                        fl = []
                        for i in range(8):
                            hd = hd_(i)[0]
                            fl.append(MM(o_(5, i), MrbT[:, i * 64:(i + 1) * 64], U[:, i * 64:(i + 1) * 64], start=True, stop=False))
                            fl.append(MM(o_(5, i), MrkT[:, i * 64:(i + 1) * 64], Vtm[:, hd * 64:(hd + 1) * 64], start=False, stop=True))
                        P.pe(fl, reads=["MrbT", "MrkT", "W0", "Vtm"], writes=[("ps", 5)])
                        TT_(Ysb[:, 512 * hg:512 * hg + 512], Ysb[:, 512 * hg:512 * hg + 512], psum[5][0:64, 0:512], ALU.add, ["Ysb", ("ps", 5)], ["Ysb"])
                        fl = []
                        for bi in range(4):
                            b = 4 * hg + bi
                            fl.append(MM(psum[3][:, bi * 128:(bi + 1) * 128], bhtm[:, b * 128:(b + 1) * 128], U[:, bi * 128:(bi + 1) * 128], start=True, stop=False))
                            fl.append(MM(psum[3][:, bi * 128:(bi + 1) * 128], khtm[:, b * 128:(b + 1) * 128], Vtm[:, b * 128:(b + 1) * 128], start=False, stop=True))
                        P.pe(fl, reads=["bhtm", "khtm", "W0", "Vtm"], writes=[("ps", 3)])
                        for bi in range(4):
                            b = 4 * hg + bi
                            for hf in range(2):
                                rows = slice(64 * hf, 64 * hf + 64)
                                STT_(rH[rows, b, :], rH[rows, b, :], epos[rows, b, 63:64], psum[3][rows, bi * 128 + hf * 64:bi * 128 + hf * 64 + 64],
                                     ALU.mult, ALU.add, ["rH", "epos", ("ps", 3)], ["rH"])
                    Y3 = Ysb[:].rearrange("p (h d) -> p h d", h=16)
                    P.op("dve", lambda e: e.tensor_reduce(out=st16[:], in_=Y3, axis=AX.X, op=ALU.add), reads=["Ysb"], writes=["st16"])
                    TS_(st16[:], st16[:], 1.0 / 64, None, ALU.mult, None, ["st16"], ["st16"])
                    yc3 = yc[:].rearrange("p (h d) -> p h d", h=16)
                    TT_(yc3, Y3, bc3(st16[:], 64), ALU.subtract, ["Ysb", "st16"], ["khtm"])
                    yt3 = ytm[:].rearrange("p (h d) -> p h d", h=16)
                    TT_(yt3, yc3, yc3, ALU.mult, ["khtm"], ["bhtm"])
                    P.op("dve", lambda e: e.tensor_reduce(out=st16b[:], in_=yt3, axis=AX.X, op=ALU.add), reads=["bhtm"], writes=["st16b"])
                    TS_(st16b[:], st16b[:], 1.0 / 64, 64e-5, ALU.mult, ALU.add, ["st16b"], ["st16b"])
                    ACT_(st16b[:], st16b[:], AF.Sqrt, ["st16b"], ["st16b"])
                    RECIP_(st16b[:], st16b[:], ["st16b"], ["st16b"])
                    TT_(yt3, yc3, bc3(st16b[:], 64), ALU.mult, ["khtm", "st16b", "bhtm"], ["bhtm"])
                    TT_(ytm[:], ytm[:], lnw[:], ALU.mult, ["bhtm", "lnw"], ["bhtm"])
                    TT_(ytm[:], ytm[:], lnb[:], ALU.add, ["bhtm", "lnb"], ["bhtm"])
                    TT_(yc3, Vtm[:].rearrange("p (h d) -> p h d", h=16), bc3(bon[:], 64), ALU.mult, ["Vtm", "bon", "khtm"], ["khtm"])
                    TT_(ytm[:], ytm[:], yc[:], ALU.add, ["bhtm", "khtm"], ["bhtm"])
                    TT_(ytm[:], ytm[:], gtm[:], ALU.mult, ["bhtm", "gtm"], ["bhtm"])
                    finish_branch(None, ytm, 1, c, key="bhtm")
                P.barrier()

            with contextlib.ExitStack() as S3:
                def a_(name, shape, dt=F32):
                    return S3.enter_context(nc.sbuf_tensor(uname(name), list(shape), dt))
                zb = a_("zbg", [128, 25, TM + 4])
                P.op("dve", lambda e: e.memset(zb[:], 0.0), writes=["zb"])
                gk = a_("gk", [16, 512]); bcn = a_("bcng", [64, 1024])
                la = a_("la", [128, 4, TM])
                bcs = a_("bcs", [128, 64]); e1 = a_("e1", [128, 64]); e2 = a_("e2", [128, 64])
                qt = a_("qt", [128, 64], BF16); kf = a_("kf", [128, 64]); ktb = a_("ktb", [128, 64], BF16); khf = a_("khf", [128, 64])
                khat = a_("khatg", [64, 128], BF16); ptb = a_("ptbg", [64, 64], BF16)
                vtm = a_("vtmg", [64, 1024], BF16); sgo = a_("sgo", [64, 1024]); obuf = a_("obuf", [64, 4, 256]); ytm = a_("ytmg", [64, 1024])
                sc = dict(ss=a_("ssg", [64, 16]), junk=a_("junkg", [64, 256]))
                P.dma("sp", gk[:], smat["gk"][:, :], "gk", writes=["gk"])
                P.dma("sp", bcn[:], bcast_in[l, :, 1024:2048], "bcn", writes=["bcn"])
                zblocks(zb, 53, 78, 3)
                for h in range(4):
                    P.pe([MM(psum[3][:, 0:TM], gk[:, h * 128:(h + 1) * 128], zb[0:16, 16, 3:3 + TM])], reads=["gk", "zb"], writes=[("ps", 3)])
                    ACT_(la[:, h, :], psum[3][:, 0:TM], AF.Sigmoid, [("ps", 3), "cols"], ["la"], bias=cols[:, OFF["g_bias"] + h:OFF["g_bias"] + h + 1])
                    ACT_(la[:, h, :], la[:, h, :], AF.Ln, ["la"], ["la"])
                for c in range(NCH_G):
                    cs = slice(64 * c, 64 * c + 64)
                    zs = slice(3 + 64 * c, 3 + 64 * c + 64)
                    to_tm(zb, 8, zs, lambda hf, ps_, r, w: CP_(vtm[:, 512 * hf:512 * hf + 512], ps_, r, w, eng="act"), "vtm")
                    to_tm(zb, 17, zs, lambda hf, ps_, r, w: ACT_(sgo[:, 512 * hf:512 * hf + 512], ps_, AF.Silu, r, w), "gtm")
                    for h in range(4):
                        SCAN_(bcs[:], onesf[:, 0:64], la[:, h, cs], 0.0, ALU.mult, ALU.add, ["la", "consts"], ["bcs"])
                        ACT_(e1[:], bcs[:], AF.Exp, ["bcs"], ["e1"], scale=1.0 / 16)
                        ACT_(e2[:], bcs[:], AF.Exp, ["bcs"], ["e2"], scale=-1.0 / 16)
                        STT_(qt[:], zb[:, h, zs], DKS, e1[:], ALU.mult, ALU.mult, ["zb", "e1"], ["qt"])
                        TT_(kf[:], zb[:, 4 + h, zs], e2[:], ALU.mult, ["zb", "e2"], ["kf"])
                        CP_(ktb[:], kf[:], ["kf"], ["ktb"], eng="act")
                        TS_(khf[:], kf[:], e1[:, 63:64], None, ALU.mult, None, ["kf", "e1"], ["khf"])
                        P.pe([TR(psum[6][0:64, 0:128], khf[:], ident), MM(psum[6][0:64, 128:192], ktb[:], qt[:])],
                             reads=["khf", "ktb", "qt", "consts"], writes=[("ps", 6)])
                        CP_(khat[:], psum[6][0:64, 0:128], [("ps", 6)], ["khat"])
                        TT_(ptb[:], psum[6][0:64, 128:192], incl, ALU.mult, [("ps", 6), "consts"], ["ptb"])
                        P.pe([MM(psum[7][0:64, 0:256], ptb[:], vtm[:, h * 256:(h + 1) * 256], start=True, stop=False),
                              MM(psum[7][0:64, 0:256], qt[:], gSb[:, h, :], start=False, stop=True)],
                             reads=["ptb", "vtm", "qt", "gSb"], writes=[("ps", 7)])
                        CP_(obuf[:, h, :], psum[7][0:64, 0:256], [("ps", 7)], ["hbuf"], eng="act")
                        P.pe([MM(psum[6][:, 0:256], khat[:], vtm[:, h * 256:(h + 1) * 256])], reads=["khat", "vtm"], writes=[("ps", 6)])
                        STT_(gS[:, h, :], gS[:, h, :], e1[:, 63:64], psum[6][:, 0:256], ALU.mult, ALU.add, ["gS", "e1", ("ps", 6)], ["gS"])
                        CP_(gSb[:, h, :], gS[:, h, :], ["gS"], ["gSb"], eng="act")
                    head_norm_gate(sc, obuf, 4, 256, bcn, sgo, ytm, EPS)
                    finish_branch(sc, ytm, 2, c)
                P.barrier()

            with contextlib.ExitStack() as S4:
                def a_(name, shape, dt=F32):
                    return S4.enter_context(nc.sbuf_tensor(uname(name), list(shape), dt))
                mT = a_("mT", [128, KC, TM], BF16)
                wg = [a_("wg%d" % i, [128, 3, KC * 128], BF16) for i in range(2)]
                wb = [a_("wb%d" % i, [128, 3, 1024], BF16) for i in range(2)]
                sgt = [a_("sgt%d" % i, [128, TM]) for i in range(3)]
                macc = a_("macc", [128, TM]); mtmp = a_("mtmp", [128, TM])
                xr = [a_("xrm%d" % i, [128, TM]) for i in range(2)]
                for d in range(KC):
                    s = d % 2
                    P.dma("sp", wg[s][:], wblocks(bn_b, 3 * d, 3), ("wg", s), reads=[("gat", bn_b)], writes=[("wg", s)])
                    P.dma("sp", wb[s][:], wblocks(bn_c, 3 * d, 3), ("wb", s), reads=[("gat", bn_c)], writes=[("wb", s)])
                    for i in range(3):
                        P.mm([(psum[1 + i][:, 0:TM], wg[s][:, i, kc * 128:(kc + 1) * 128], hT[:, kc, :], kc == 0, kc == KC - 1) for kc in range(KC)],
                             reads=[("wg", s), "hT"], writes=[("ps", 1 + i)])
                        ACT_(sgt[i][:], psum[1 + i][:, 0:TM], AF.Sigmoid, [("ps", 1 + i)], [("sgt", i)])
                        P.mm([(psum[4 + i][:, 0:TM], wb[s][:, i, kc * 128:(kc + 1) * 128], yT[:, 8 * i + kc, :], kc == 0, kc == 7) for kc in range(8)],
                             reads=[("wb", s), "yT"], writes=[("ps", 4 + i)])
                    TT_(macc[:], sgt[0][:], psum[4][:, 0:TM], ALU.mult, [("sgt", 0), ("ps", 4)], ["macc"])
                    TT_(mtmp[:], sgt[1][:], psum[5][:, 0:TM], ALU.mult, [("sgt", 1), ("ps", 5)], ["mtmp"])
                    TT_(macc[:], macc[:], mtmp[:], ALU.add, ["macc", "mtmp"], ["macc"])
                    TT_(mtmp[:], sgt[2][:], psum[6][:, 0:TM], ALU.mult, [("sgt", 2), ("ps", 6)], ["mtmp"])
                    TT_(mT[:, d, :], macc[:], mtmp[:], ALU.add, ["macc", "mtmp"], ["mT"])
                for d in range(KC):
                    s = nwz[0] % 2; nwz[0] += 1
                    par = 1 + s
                    P.dma("sp", wz[s][:], wblocks(bn_d, d, 1)[:, 0, :], ("wz", s), reads=[("gat", bn_d)], writes=[("wz", s)])
                    P.mm([(psum[par][:, 0:TM], wz[s][:, kc * 128:(kc + 1) * 128], mT[:, kc, :], kc == 0, kc == KC - 1) for kc in range(KC)],
                         reads=[("wz", s), "mT"], writes=[("ps", par)])
                    sx = d % 2
                    P.dma("sp", xr[sx][:], xT_ap(d, t0, TM), ("xrm", sx), reads=[("xT", xkey)], writes=[("xrm", sx)])
                    TT_(xr[sx][:], xr[sx][:], psum[par][:, 0:TM], ALU.add, [("xrm", sx), ("ps", par)], [("xrm", sx)])
                    P.dma("sp", xT_ap(d, t0, TM), xr[sx][:], ("xrmst", sx), reads=[("xrm", sx)], writes=[("xT", xkey)])
                P.barrier()
        P.barrier()
```
